# Optimizing a Trainium2 kernel written in Bass

```python
import math
import jax, jax.numpy as jnp
from jax import lax
import numpy as np

D_MODEL = 2048
BATCH = 4
SEQ = 2048
DEPTH = 1

ATTN_HEADS = 8
HEAD_DIM = 128
ATTN_WIDTH = ATTN_HEADS * HEAD_DIM
REC_WIDTH = D_MODEL - ATTN_WIDTH
MIX_WIDTH = ATTN_WIDTH + REC_WIDTH
REC_BLOCKS = 8
REC_BLOCK = REC_WIDTH // REC_BLOCKS
CONV_WIDTH = 4
LRU_C = 8.0
IN_COLS = 3 * ATTN_WIDTH + 2 * REC_WIDTH
DILATED_PATTERNS = ((128, 1), (512, 4), (2048, 16))
ROPE_THETA = 10000.0
NEG_INF = -1e30
PEER_HEADS = 8
PEER_NKEYS = 128
PEER_EXPERTS = PEER_NKEYS * PEER_NKEYS
PEER_QDIM = 256
PEER_HALF = PEER_QDIM // 2
PEER_TOPK = 16
PEER_CHUNK = 128
DN_ALPHA = (2.0 * DEPTH) ** 0.25
DN_BETA = (8.0 * DEPTH) ** -0.25
LN_EPS = 1e-5

kernel_name = 'hymba_rglru_dilated_attn_peer'


def layer_norm(x, g, b):
    xf = x.astype(jnp.float32)
    mu = jnp.mean(xf, -1, keepdims=True)
    var = jnp.mean(jnp.square(xf - mu), -1, keepdims=True)
    return ((xf - mu) * lax.rsqrt(var + LN_EPS) * g.astype(jnp.float32) + b.astype(jnp.float32)).astype(x.dtype)


def rms_norm(x, g):
    xf = x.astype(jnp.float32)
    return xf * lax.rsqrt(jnp.mean(jnp.square(xf), -1, keepdims=True) + LN_EPS) * g.astype(jnp.float32)


def rope(x, positions):
    half = HEAD_DIM // 2
    inv = ROPE_THETA ** (-jnp.arange(half, dtype=jnp.float32) / half)
    ang = positions.astype(jnp.float32)[..., None] * inv
    cos = jnp.cos(ang)[:, :, None, :]
    sin = jnp.sin(ang)[:, :, None, :]
    x1, x2 = x[..., :half], x[..., half:]
    return jnp.concatenate([x1 * cos - x2 * sin, x2 * cos + x1 * sin], -1)


def banded_causal_attn(q, k, v, band):
    *lead, L, hd = q.shape
    nb = -(-L // band)
    lp = nb * band
    nlead = len(lead)
    qb = jnp.pad(q, [(0, 0)] * nlead + [(0, lp - L), (0, 0)]).reshape(*lead, nb, band, hd)
    kvpad = [(0, 0)] * nlead + [(band, lp - L), (0, 0)]
    kb = jnp.pad(k, kvpad).reshape(*lead, nb + 1, band, hd)
    vb = jnp.pad(v, kvpad).reshape(*lead, nb + 1, band, hd)
    kw = jnp.concatenate([kb[..., :-1, :, :], kb[..., 1:, :, :]], axis=-2)
    vw = jnp.concatenate([vb[..., :-1, :, :], vb[..., 1:, :, :]], axis=-2)
    s = jnp.einsum('...nqd,...nkd->...nqk', qb, kw)
    qi = jnp.arange(band)[:, None]
    kj = jnp.arange(2 * band)[None, :]
    dist = band + qi - kj
    kpos = jnp.arange(nb)[:, None, None] * band - band + kj
    valid = (dist >= 0) & (dist <= band) & (kpos >= 0)
    s = jnp.where(valid, s, NEG_INF)
    m = jnp.max(s, -1)
    p = jnp.exp(s - m[..., None])
    l = jnp.sum(p, -1)
    acc = jnp.einsum('...nqk,...nkd->...nqd', p, vw)
    return (acc.reshape(*lead, lp, hd)[..., :L, :],
            m.reshape(*lead, lp)[..., :L],
            l.reshape(*lead, lp)[..., :L])


def dilated_attention(q, k, v):
    B, S, H, hd = q.shape
    accs, ms, ls = [], [], []
    for window, dil in DILATED_PATTERNS:
        L = S // dil
        def split(t):
            return t.reshape(B, L, dil, H, hd).transpose(0, 2, 3, 1, 4)
        acc, m, l = banded_causal_attn(split(q), split(k), split(v), window // dil)
        accs.append(acc.transpose(0, 3, 1, 2, 4).reshape(B, S, H, hd))
        ms.append(m.transpose(0, 3, 1, 2).reshape(B, S, H))
        ls.append(l.transpose(0, 3, 1, 2).reshape(B, S, H))
    m_all = jnp.stack(ms)
    wts = jnp.exp(m_all - jnp.max(m_all, 0))
    num = jnp.einsum('pbsh,pbshd->bshd', wts, jnp.stack(accs))
    den = jnp.sum(wts * jnp.stack(ls), 0)
    return num / den[..., None]


def causal_depthwise_conv(x, w, bias):
    C = x.shape[-1]
    y = lax.conv_general_dilated(x, w.astype(x.dtype)[:, None, :], window_strides=(1,),
                                 padding=[(CONV_WIDTH - 1, 0)],
                                 dimension_numbers=('NWC', 'WIO', 'NWC'),
                                 feature_group_count=C)
    return y + bias.astype(x.dtype)


def rg_lru(x, w_a, b_a, w_x, b_x, lam):
    B, S, R = x.shape
    xb = x.reshape(B, S, REC_BLOCKS, REC_BLOCK)
    r = jax.nn.sigmoid(jnp.einsum('bsnc,ncd->bsnd', xb, w_a.astype(jnp.float32)).reshape(B, S, R) + b_a.astype(jnp.float32))
    i = jax.nn.sigmoid(jnp.einsum('bsnc,ncd->bsnd', xb, w_x.astype(jnp.float32)).reshape(B, S, R) + b_x.astype(jnp.float32))
    log_a = -LRU_C * r * jax.nn.softplus(-lam.astype(jnp.float32))
    a = jnp.exp(log_a)
    bterm = jnp.sqrt(-jnp.expm1(2.0 * log_a)) * (i * x)
    def combine(left, right):
        a1, b1 = left
        a2, b2 = right
        return a1 * a2, a2 * b1 + b2
    _, h = lax.associative_scan(combine, (a, bterm), axis=1)
    return h


def peer(x, wq, keys1, keys2, u, v):
    B, S, D = x.shape
    T = B * S
    xt = x.reshape(T, D)
    q = (xt @ wq).astype(jnp.float32).reshape(T, PEER_HEADS, 2, PEER_HALF)
    s1 = jnp.einsum('thd,kd->thk', q[:, :, 0], keys1.astype(jnp.float32))
    s2 = jnp.einsum('thd,kd->thk', q[:, :, 1], keys2.astype(jnp.float32))
    v1, i1 = lax.top_k(s1, PEER_TOPK)
    v2, i2 = lax.top_k(s2, PEER_TOPK)
    cand_s = (v1[..., :, None] + v2[..., None, :]).reshape(T, PEER_HEADS, PEER_TOPK * PEER_TOPK)
    cand_i = (i1[..., :, None] * PEER_NKEYS + i2[..., None, :]).reshape(T, PEER_HEADS, PEER_TOPK * PEER_TOPK)
    top_s, pos = lax.top_k(cand_s, PEER_TOPK)
    idx = jnp.take_along_axis(cand_i, pos, axis=-1).reshape(T, PEER_HEADS * PEER_TOPK)
    gates = jax.nn.softmax(top_s, axis=-1).reshape(T, PEER_HEADS * PEER_TOPK).astype(x.dtype)
    n_chunks = T // PEER_CHUNK
    E = PEER_HEADS * PEER_TOPK
    def chunk_fn(args):
        xc, ic, gc = args
        uc = jnp.take(u, ic, axis=0)
        act = jax.nn.gelu(jnp.einsum('cd,ced->ce', xc, uc))
        vc = jnp.take(v, ic, axis=0)
        return jnp.einsum('ce,ced->cd', gc * act, vc)
    out = lax.map(chunk_fn, (xt.reshape(n_chunks, PEER_CHUNK, D),
                             idx.reshape(n_chunks, PEER_CHUNK, E),
                             gates.reshape(n_chunks, PEER_CHUNK, E)))
    return out.reshape(B, S, D)


def setup_inputs(seed: int = 0) -> dict:
    key = jax.random.key(seed)
    ks = jax.random.split(key, 24)
    f32 = jnp.float32
    nrm = lambda k, shape, s: jax.random.normal(k, shape, f32) * s
    x = jax.random.normal(ks[0], (BATCH, SEQ, D_MODEL), f32)
    positions = jnp.broadcast_to(jnp.arange(SEQ, dtype=jnp.int32), (BATCH, SEQ))
    col_scale = jnp.concatenate([jnp.ones((2 * ATTN_WIDTH,), f32),
                                 jnp.full((ATTN_WIDTH,), DN_BETA, f32),
                                 jnp.ones((2 * REC_WIDTH,), f32)])
    w_in = nrm(ks[1], (DEPTH, D_MODEL, IN_COLS), D_MODEL ** -0.5) * col_scale
    conv_w = nrm(ks[2], (DEPTH, CONV_WIDTH, REC_WIDTH), CONV_WIDTH ** -0.5)
    conv_b = nrm(ks[3], (DEPTH, REC_WIDTH), 0.01)
    rg_w_a = nrm(ks[4], (DEPTH, REC_BLOCKS, REC_BLOCK, REC_BLOCK), REC_BLOCK ** -0.5)
    rg_b_a = nrm(ks[5], (DEPTH, REC_WIDTH), 0.01)
    rg_w_x = nrm(ks[6], (DEPTH, REC_BLOCKS, REC_BLOCK, REC_BLOCK), REC_BLOCK ** -0.5)
    rg_b_x = nrm(ks[7], (DEPTH, REC_WIDTH), 0.01)
    a_c = jax.random.uniform(ks[8], (DEPTH, REC_WIDTH), f32, 0.9, 0.999)
    a0 = a_c ** (1.0 / LRU_C)
    lru_lambda = jnp.log(a0) - jnp.log1p(-a0)
    gn_attn = 1.0 + nrm(ks[9], (DEPTH, ATTN_WIDTH), 0.01)
    gn_rec = 1.0 + nrm(ks[10], (DEPTH, REC_WIDTH), 0.01)
    w_out = nrm(ks[11], (DEPTH, MIX_WIDTH, D_MODEL), MIX_WIDTH ** -0.5 * DN_BETA)
    ln1_g = 1.0 + nrm(ks[12], (DEPTH, D_MODEL), 0.01)
    ln1_b = nrm(ks[13], (DEPTH, D_MODEL), 0.01)
    peer_wq = nrm(ks[14], (DEPTH, D_MODEL, PEER_HEADS * PEER_QDIM), D_MODEL ** -0.5)
    peer_keys1 = nrm(ks[15], (DEPTH, PEER_NKEYS, PEER_HALF), PEER_HALF ** -0.5)
    peer_keys2 = nrm(ks[16], (DEPTH, PEER_NKEYS, PEER_HALF), PEER_HALF ** -0.5)
    peer_u = nrm(ks[17], (DEPTH, PEER_EXPERTS, D_MODEL), D_MODEL ** -0.5 * DN_BETA)
    peer_v = nrm(ks[18], (DEPTH, PEER_EXPERTS, D_MODEL), DN_BETA)
    ln2_g = 1.0 + nrm(ks[19], (DEPTH, D_MODEL), 0.01)
    ln2_b = nrm(ks[20], (DEPTH, D_MODEL), 0.01)
    return {'x': x, 'positions': positions, 'w_in': w_in, 'conv_w': conv_w, 'conv_b': conv_b,
            'rg_w_a': rg_w_a, 'rg_b_a': rg_b_a, 'rg_w_x': rg_w_x, 'rg_b_x': rg_b_x,
            'lru_lambda': lru_lambda, 'gn_attn': gn_attn, 'gn_rec': gn_rec, 'w_out': w_out,
            'ln1_g': ln1_g, 'ln1_b': ln1_b, 'peer_wq': peer_wq, 'peer_keys1': peer_keys1,
            'peer_keys2': peer_keys2, 'peer_u': peer_u, 'peer_v': peer_v,
            'ln2_g': ln2_g, 'ln2_b': ln2_b}


def reference(x, positions, w_in, conv_w, conv_b, rg_w_a, rg_b_a, rg_w_x, rg_b_x, lru_lambda,
              gn_attn, gn_rec, w_out, ln1_g, ln1_b, peer_wq, peer_keys1, peer_keys2,
              peer_u, peer_v, ln2_g, ln2_b):
    B, S, _ = x.shape
    h = x
    for l in range(DEPTH):
        proj = h @ w_in[l]
        q, k, v, xr, gr = jnp.split(proj, [ATTN_WIDTH, 2 * ATTN_WIDTH, 3 * ATTN_WIDTH,
                                           3 * ATTN_WIDTH + REC_WIDTH], axis=-1)
        q = rope(q.astype(jnp.float32).reshape(B, S, ATTN_HEADS, HEAD_DIM), positions) * (HEAD_DIM ** -0.5)
        k = rope(k.astype(jnp.float32).reshape(B, S, ATTN_HEADS, HEAD_DIM), positions)
        v = v.astype(jnp.float32).reshape(B, S, ATTN_HEADS, HEAD_DIM)
        attn = dilated_attention(q, k, v).reshape(B, S, ATTN_WIDTH)
        xc = causal_depthwise_conv(xr, conv_w[l], conv_b[l]).astype(jnp.float32)
        rec = rg_lru(xc, rg_w_a[l], rg_b_a[l], rg_w_x[l], rg_b_x[l], lru_lambda[l]) \
            * jax.nn.gelu(gr.astype(jnp.float32))
        mix = jnp.concatenate([rms_norm(attn, gn_attn[l]), rms_norm(rec, gn_rec[l])], -1).astype(h.dtype)
        h = layer_norm(DN_ALPHA * h + mix @ w_out[l], ln1_g[l], ln1_b[l])
        y = peer(h, peer_wq[l], peer_keys1[l], peer_keys2[l], peer_u[l], peer_v[l])
        h = layer_norm(DN_ALPHA * h + y, ln2_g[l], ln2_b[l])
    return h
```

```python
import math
import numpy as np
import concourse.bass as bass
import concourse.mybir as mybir
from concourse.bass_utils import run_bass_kernel_spmd

F32 = mybir.dt.float32
BF16 = mybir.dt.bfloat16
I32 = mybir.dt.int32
AF = mybir.ActivationFunctionType
ALU = mybir.AluOpType
AX = mybir.AxisListType

ALPHA = 2.0 ** 0.25
EPS = 1e-5
NEG = -30000.0
PI = math.pi
NV = 88
NCM = 832
ARENA = 51600
TOKB = 256
GE = 4
DEBUG_STAGE = None


class Prog:
    def __init__(self, nc):
        self.nc = nc
        self.E = {"pe": nc.tensor, "dve": nc.vector, "act": nc.scalar, "pool": nc.gpsimd, "sp": nc.sync}
        self.S = {e: nc.alloc_semaphore("s_" + e) for e in ("pe", "dve", "act", "pool")}
        self.cnt = {e: 0 for e in self.S}
        self.NDS = 24
        self.dsem = {q: [nc.alloc_semaphore("d%s%d" % (q, i)) for i in range(self.NDS)] for q in ("sp", "pool")}
        self.nd = {"sp": 0, "pool": 0}
        self.waited = {e: {} for e in self.E}
        self.lastw = {}
        self.readers = {}
        self.out_toks = []

    def _wait(self, eng, tok):
        sem, val = tok
        if self.waited[eng].get(sem.num, 0) >= val:
            return
        self.E[eng].wait_ge(sem, val)
        self.waited[eng][sem.num] = val

    def _deps(self, eng, reads, writes):
        toks = []
        for k in reads:
            w = self.lastw.get(k)
            if w is not None:
                toks.append(w)
        for k in writes:
            w = self.lastw.get(k)
            if w is not None:
                toks.append(w)
            toks.extend(self.readers.get(k, {}).values())
        for t in toks:
            if eng == "pe" and t[0] is self.S["pe"]:
                continue
            self._wait(eng, t)

    def _commit(self, tok, reads, writes):
        for k in reads:
            r = self.readers.setdefault(k, {})
            old = r.get(tok[0].num)
            if old is None or old[1] < tok[1]:
                r[tok[0].num] = tok
        for k in writes:
            self.lastw[k] = tok
            self.readers[k] = {}

    def op(self, eng, fn, reads=(), writes=(), inc=True):
        psr = [k for k in reads if k.startswith("ps")]
        if psr:
            writes = list(writes) + psr
        self._deps(eng, reads, writes)
        inst = fn(self.E[eng])
        if inc:
            self.cnt[eng] += 1
            tok = (self.S[eng], self.cnt[eng])
            inst.then_inc(self.S[eng], 1)
        else:
            tok = (self.S[eng], self.cnt[eng] + 1)
        self._commit(tok, reads, writes)

    def dma(self, eng, out, in_, reads=(), writes=(), is_out=False):
        n = self.nd[eng]
        self.nd[eng] += 1
        sem = self.dsem[eng][n % self.NDS]
        val = 16 * (n // self.NDS + 1)
        if n >= self.NDS:
            self._wait(eng, (sem, val - 16))
        self._deps(eng, reads, writes)
        self.E[eng].dma_start(out=out, in_=in_).then_inc(sem, 16)
        self._commit((sem, val), reads, writes)
        if is_out:
            self.out_toks.append((sem, val))

    def barrier(self):
        toks = [(self.S[e], self.cnt[e]) for e in self.S if self.cnt[e] > 0]
        for q in ("sp", "pool"):
            for i in range(min(self.nd[q], self.NDS)):
                last_n = ((self.nd[q] - 1 - i) // self.NDS) * self.NDS + i
                toks.append((self.dsem[q][i], 16 * (last_n // self.NDS + 1)))
        for e in self.E:
            for t in toks:
                if e == "pe" and t[0] is self.S["pe"]:
                    continue
                self._wait(e, t)


class _Stop(Exception):
    pass


def build():
    try:
        return _build()
    except _Stop as e:
        return e.args[0]


def _build():
    nc = bass.Bass("TRN2", target_bir_lowering=False)
    d = {}

    def din(name, shape, dt=F32):
        d[name] = nc.dram_tensor(name, shape, dt, kind="ExternalInput").ap()

    din("xT", [2048, 2048]); din("xo", [1024, 2048]); din("pos", [1, 2048], I32)
    din("win", [40, 128, 2048]); din("wout", [4, 128, 8192]); din("wqk", [16, 128, 2048])
    if DEBUG_STAGE is None:
        din("uk", [128, 128, 2048]); din("v", [16384, 2048])
    din("kT", [128, 256])
    din("rgw", [128, 2048]); din("pvec", [128, NV]); din("prow", [1, 8192]); din("cmat", [128, NCM])
    y = nc.dram_tensor("y", [1024, 2048], F32, kind="ExternalOutput").ap()

    P = Prog(nc)
    arena_cm = nc.sbuf_tensor("arena", [128, ARENA], F32)
    arena = arena_cm.__enter__()
    ps_cms = [nc.psum_tensor("ps%d" % i, [128, 512], F32) for i in range(8)]
    ps = [c.__enter__()[:, :] for c in ps_cms]
    h1s = nc.dram_tensor("h1s", [1024, 2048], F32).ap()

    st = {"off": 0}

    def alloc(n, dt=F32):
        words = n if dt != BF16 else (n + 1) // 2
        words = (words + 7) // 8 * 8
        o = st["off"]
        assert o + words <= ARENA, ("arena overflow", o, words)
        st["off"] = o + words
        a = arena[:, o:o + words]
        if dt == BF16:
            a = a.bitcast(BF16)[:, 0:n]
        elif dt == I32:
            a = a.bitcast(I32)[:, 0:n]
        else:
            a = a[:, 0:n]
        return a

    def dve(fn, r=(), w=()):
        P.op("dve", fn, r, w)

    def actop(out, in_, func, r=(), w=(), scale=1.0, bias=0.0):
        P.op("act", lambda e: e.activation(out=out, in_=in_, func=func, scale=scale, bias=bias), r, w)

    def mm(out, lhsT, rhs, start, stop, r=(), w=(), inc=True):
        P.op("pe", lambda e: e.matmul(out, lhsT=lhsT, rhs=rhs, start=start, stop=stop), r, w, inc=inc)

    def ckpt(tag, items):
        if DEBUG_STAGE != tag:
            return
        P.barrier()
        for (ap, key, row0) in items:
            n = ap.shape[1]
            P.dma("pool", y[row0:row0 + 128, 0:n], ap, reads=[key], is_out=True)
        for t in P.out_toks:
            P._wait("sp", t)
        raise _Stop(nc)

    pv = alloc(NV)
    P.dma("sp", pv, d["pvec"], writes=["pv"])
    cbt = alloc(NCM, BF16)
    P.dma("pool", cbt, d["cmat"], writes=["cb"])
    ident = cbt[:, 0:128]; prot = cbt[:, 128:256]; ones = cbt[:, 256:384]
    nm_cur = cbt[:, 384:512]; nm_prev = cbt[:, 512:640]; nm_pf = cbt[:, 640:768]; nm16 = cbt[:, 768:832]
    kT32 = alloc(256)
    P.dma("sp", kT32, d["kT"], writes=["kT32"])
    nsp = alloc(16)
    tmp8 = alloc(8)
    actop(tmp8, pv[:, 24:32], AF.Exp, r=["pv"], w=["tmp8"], scale=-1.0)
    actop(tmp8, tmp8, AF.Ln, r=["tmp8"], w=["tmp8"], bias=1.0)
    dve(lambda e: e.tensor_scalar(out=nsp[:, 0:8], in0=tmp8, scalar1=-8.0, scalar2=None, op0=ALU.mult), ["tmp8"], ["nsp"])
    dve(lambda e: e.tensor_scalar(out=nsp[:, 8:16], in0=tmp8, scalar1=-16.0, scalar2=None, op0=ALU.mult), ["tmp8"], ["nsp"])
    mark0 = st["off"]

    mixT = alloc(16 * 1024, BF16).rearrange("p (c t) -> p c t", c=16)
    xTb = alloc(16 * 2048, BF16).rearrange("p (c t) -> p c t", c=16)
    for c in range(16):
        P.dma("pool", xTb[:, c, :], d["xT"][c * 128:(c + 1) * 128, :], writes=["xT%d" % c])
    wr = [alloc(2048, BF16).rearrange("p (c j) -> p c j", c=16) for _ in range(3)]
    wseq = []
    for j in range(8):
        wseq += [24 + j, 32 + j]
    for h in range(8):
        wseq += [h, 8 + h, 16 + h]
    wstate = {"next": 0}

    def wload_upto(i):
        while wstate["next"] <= min(i, len(wseq) - 1):
            k = wstate["next"]
            P.dma("pool", wr[k % 3].rearrange("p c j -> p (c j)"), d["win"][wseq[k]], writes=["w%d" % (k % 3)])
            wstate["next"] += 1

    def proj(i, tok0, ntb, banks):
        wload_upto(i + 2)
        w = wr[i % 3]
        for tb in range(ntb):
            for c in range(16):
                mm(ps[banks[tb]], w[:, c, :], xTb[:, c, tok0 + tb * 512: tok0 + (tb + 1) * 512], c == 0, c == 15,
                   r=["w%d" % (i % 3), "xT%d" % c], w=["ps%d" % banks[tb]], inc=(c == 15))

    cosT = alloc(2048); sinT = alloc(2048)
    mark1 = st["off"]
    posi = alloc(2048, I32)
    P.dma("sp", posi, d["pos"].to_broadcast([128, 2048]), writes=["posi"])
    ang = alloc(2048)
    angi = alloc(2048, I32); frac = alloc(2048); cmp = alloc(2048)
    TWO_PI_S = 6.28318
    dve(lambda e: e.tensor_copy(out=ang, in_=posi), ["posi"], ["ang"])
    dve(lambda e: e.tensor_scalar(out=ang, in0=ang, scalar1=pv[:, 80:81], scalar2=1.0 / (2 * PI), op0=ALU.mult, op1=ALU.mult), ["ang", "pv"], ["ang"])
    dve(lambda e: e.tensor_copy(out=angi, in_=ang), ["ang"], ["angi"])
    dve(lambda e: e.tensor_copy(out=frac, in_=angi), ["angi"], ["frac"])
    dve(lambda e: e.tensor_tensor(out=frac, in0=ang, in1=frac, op=ALU.subtract), ["ang", "frac"], ["frac"])

    def wrap(buf, key):
        dve(lambda e: e.tensor_scalar(out=cmp, in0=buf, scalar1=0.5, scalar2=None, op0=ALU.is_gt), [key], ["cmp"])
        dve(lambda e: e.tensor_tensor(out=buf, in0=buf, in1=cmp, op=ALU.subtract), [key, "cmp"], [key])
        dve(lambda e: e.tensor_scalar(out=cmp, in0=buf, scalar1=-0.5, scalar2=None, op0=ALU.is_lt), [key], ["cmp"])
        dve(lambda e: e.tensor_tensor(out=buf, in0=buf, in1=cmp, op=ALU.add), [key, "cmp"], [key])

    wrap(frac, "frac")
    actop(sinT, frac, AF.Sin, r=["frac", "pv"], w=["sinT"], scale=pv[:, 83:84])
    dve(lambda e: e.tensor_scalar(out=frac, in0=frac, scalar1=0.25, scalar2=None, op0=ALU.add), ["frac"], ["frac"])
    wrap(frac, "frac")
    actop(cosT, frac, AF.Sin, r=["frac"], w=["cosT"], scale=TWO_PI_S)
    P.barrier()
    st["off"] = mark1
    if DEBUG_STAGE == "s0":
        P.dma("sp", y[0:128, :], sinT, reads=["sinT"], is_out=True)
        P.dma("sp", y[128:256, :], cosT, reads=["cosT"], is_out=True)
        for t in P.out_toks:
            P._wait("sp", t)
        return nc

    rgw = alloc(2048, BF16)
    P.dma("pool", rgw, d["rgw"], writes=["rgw"])
    rgwv = rgw.rearrange("p (g n k) -> p g n k", g=2, n=8)
    xr = alloc(2052); xc = alloc(2048); xcb = alloc(2048, BF16)
    R = alloc(2048); I_ = alloc(2048); B = alloc(2048)
    xg = alloc(1024); zg = alloc(1024); sg = alloc(1024); sqrb = alloc(1024, BF16)
    rs = alloc(1024)
    P.op("pool", lambda e: e.memset(xr[:, 0:4], 0.0), [], ["xr"])
    for j in range(8):
        proj(2 * j, 0, 4, [0, 1, 2, 3])
        for tb in range(4):
            actop(xr[:, 4 + tb * 512: 4 + (tb + 1) * 512], ps[tb], AF.Identity, r=["ps%d" % tb], w=["xr"])
        cw = 48 + 4 * j
        dve(lambda e: e.tensor_scalar(out=xc, in0=xr[:, 1:2049], scalar1=pv[:, cw:cw + 1], scalar2=pv[:, j:j + 1],
                                      op0=ALU.mult, op1=ALU.add), ["xr", "pv"], ["xc"])
        for t in range(1, 4):
            dve(lambda e: e.scalar_tensor_tensor(out=xc, in0=xr[:, 1 + t:2049 + t], scalar=pv[:, cw + t:cw + t + 1],
                                                 op0=ALU.mult, in1=xc, op1=ALU.add), ["xr", "pv", "xc"], ["xc"])
        actop(xcb, xc, AF.Identity, r=["xc"], w=["xcb"])
        for g in range(2):
            dst = R if g == 0 else I_
            for tb in range(4):
                mm(ps[tb], rgwv[:, g, j, :], xcb[:, tb * 512:(tb + 1) * 512], True, True, r=["rgw", "xcb"], w=["ps%d" % tb])
            for tb in range(4):
                actop(dst[:, tb * 512:(tb + 1) * 512], ps[tb], AF.Sigmoid, r=["ps%d" % tb, "pv"], w=["R" if g == 0 else "I"],
                      bias=pv[:, 8 + 8 * g + j: 9 + 8 * g + j])
        actop(B, R, AF.Exp, r=["R", "nsp"], w=["B"], scale=nsp[:, 8 + j:9 + j])
        actop(B, B, AF.Sqrt, r=["B"], w=["B"], scale=-1.0, bias=1.0)
        actop(R, R, AF.Exp, r=["R", "nsp"], w=["R"], scale=nsp[:, j:j + 1])
        dve(lambda e: e.tensor_tensor(out=I_, in0=I_, in1=xc, op=ALU.mult), ["I", "xc"], ["I"])
        dve(lambda e: e.tensor_tensor(out=I_, in0=I_, in1=B, op=ALU.mult), ["I", "B"], ["I"])
        dve(lambda e: e.tensor_scalar(out=I_[:, 0:1024], in0=I_[:, 0:1024], scalar1=pv[:, 82:83], scalar2=None, op0=ALU.mult),
            ["I", "pv"], ["I"])
        dve(lambda e: e.tensor_tensor_scan(out=B, data0=R, data1=I_, initial=0.0, op0=ALU.mult, op1=ALU.add), ["R", "I", "B"], ["B"])
        proj(2 * j + 1, 1024, 2, [4, 5])
        for tb in range(2):
            sl = slice(tb * 512, (tb + 1) * 512)
            actop(xg[:, sl], ps[4 + tb], AF.Identity, r=["ps%d" % (4 + tb)], w=["xg"])
            actop(zg[:, sl], ps[4 + tb], AF.Square, r=["ps%d" % (4 + tb)], w=["zg"])
        dve(lambda e: e.tensor_scalar(out=zg, in0=zg, scalar1=0.044715, scalar2=1.0, op0=ALU.mult, op1=ALU.add), ["zg"], ["zg"])
        dve(lambda e: e.tensor_tensor(out=zg, in0=zg, in1=xg, op=ALU.mult), ["zg", "xg"], ["zg"])
        actop(sg, zg, AF.Sigmoid, r=["zg"], w=["sg"], scale=1.5957691216057308)
        dve(lambda e: e.tensor_tensor(out=xg, in0=xg, in1=sg, op=ALU.mult), ["xg", "sg"], ["xg"])
        dve(lambda e: e.tensor_tensor(out=xg, in0=xg, in1=B[:, 1024:2048], op=ALU.mult), ["xg", "B"], ["xg"])
        actop(mixT[:, 8 + j, :], xg, AF.Identity, r=["xg"], w=["mix%d" % (8 + j)])
        actop(sqrb, xg, AF.Square, r=["xg"], w=["sqrb"])
        for tb in range(2):
            mm(ps[6 + tb], ones, sqrb[:, tb * 512:(tb + 1) * 512], j == 0, j == 7,
               r=["cb", "sqrb"], w=["ps%d" % (6 + tb)])
    for tb in range(2):
        actop(rs[:, tb * 512:(tb + 1) * 512], ps[6 + tb], AF.Sqrt, r=["ps%d" % (6 + tb)], w=["rs"], scale=1.0 / 1024, bias=EPS)
    dve(lambda e: e.reciprocal(out=rs, in_=rs), ["rs"], ["rs"])
    for j in range(8):
        dve(lambda e: e.scalar_tensor_tensor(out=mixT[:, 8 + j, :], in0=mixT[:, 8 + j, :], scalar=pv[:, 32 + j:33 + j],
                                             op0=ALU.mult, in1=rs, op1=ALU.mult), ["mix%d" % (8 + j), "pv", "rs"], ["mix%d" % (8 + j)])
    P.barrier()
    st["off"] = mark1
    if DEBUG_STAGE == "s1a":
        for j in range(8):
            P.dma("pool", y[j * 128:(j + 1) * 128, 0:1024], mixT[:, 8 + j, :], reads=["mix%d" % (8 + j)], is_out=True)
        for t in P.out_toks:
            P._wait("sp", t)
        return nc

    kTh = alloc(2048, BF16); vTh = alloc(2048, BF16); qTh = alloc(1024, BF16)
    kT4 = alloc(2048, BF16); kT16 = alloc(2048, BF16); vT4 = alloc(2048, BF16); vT16 = alloc(2048, BF16)
    Vb = [alloc(2048, BF16).rearrange("p (b k) -> p b k", b=16) for _ in range(3)]
    qb = alloc(512, BF16); t1 = alloc(512); t2 = alloc(512)
    pb = [alloc(512, BF16) for _ in range(2)]
    num = alloc(1024); den = alloc(1024)
    SC = 128.0 ** -0.5
    ckpt("b0", [(cosT, "cosT", 0)])
    for h in range(8):
        base = 16 + 3 * h
        for (wi, tok0, ntb, dst, key) in ((base, 1024, 2, qTh, "qTh"), (base + 1, 0, 4, kTh, "kTh")):
            proj(wi, tok0, ntb, list(range(ntb)))
            ckpt("b01", [(cosT, "cosT", 0)])
            for tb in range(ntb):
                sl = slice(tb * 512, (tb + 1) * 512)
                tsl = slice(tok0 + tb * 512, tok0 + (tb + 1) * 512)
                actop(qb, ps[tb], AF.Identity, r=["ps%d" % tb], w=["qb"])
                mm(ps[5], prot, qb, True, True, r=["cb", "qb"], w=["ps5"])
                ckpt("b02", [(cosT, "cosT", 0)])
                dve(lambda e: e.tensor_tensor(out=t1, in0=ps[tb], in1=cosT[:, tsl], op=ALU.mult), ["ps%d" % tb, "cosT", "qb"], ["t1"])
                ckpt("b031", [(t1, "t1", 0)])
                ckpt("b031c", [(cosT, "cosT", 0)])
                dve(lambda e: e.tensor_tensor(out=t2, in0=ps[5], in1=sinT[:, tsl], op=ALU.mult), ["ps5", "sinT"], ["t2"])
                ckpt("b032", [(t2, "t2", 0)])
                dve(lambda e: e.tensor_tensor(out=dst[:, sl], in0=t1, in1=t2, op=ALU.add), ["t1", "t2"], [key])
                ckpt("b03", [(dst, key, 0)])
            ckpt("b05", [(qTh, "qTh", 0)])
        ckpt("b1", [(kTh, "kTh", 0), (qTh, "qTh", 128)])
        proj(base + 2, 0, 4, [0, 1, 2, 3])
        for tb in range(4):
            actop(vTh[:, tb * 512:(tb + 1) * 512], ps[tb], AF.Identity, r=["ps%d" % tb], w=["vTh"])
        dve(lambda e: e.tensor_copy(out=kT4.rearrange("p (n c j) -> p n c j", n=4, c=4), in_=kTh.rearrange("p (n j c) -> p n c j", n=4, c=4)),
            ["kTh"], ["kT4"])
        dve(lambda e: e.tensor_copy(out=kT16.rearrange("p (c j) -> p c j", c=16), in_=kTh.rearrange("p (j c) -> p c j", c=16)), ["kTh"], ["kT16"])
        actop(vT4.rearrange("p (n c j) -> p n c j", n=4, c=4), vTh.rearrange("p (n j c) -> p n c j", n=4, c=4), AF.Identity, r=["vTh"], w=["vT4"])
        actop(vT16.rearrange("p (c j) -> p c j", c=16), vTh.rearrange("p (j c) -> p c j", c=16), AF.Identity, r=["vTh"], w=["vT16"])
        ksrc = {1: (kTh, "kTh"), 4: (kT4, "kT4"), 16: (kT16, "kT16")}
        vsrc = {1: (vTh, "vTh"), 4: (vT4, "vT4"), 16: (vT16, "vT16")}

        def ksel(dil, blk):
            return slice(blk * 128, (blk + 1) * 128)
        for di, dil in enumerate((1, 4, 16)):
            for g in range(4):
                for i in range(4):
                    mm(ps[4][:, i * 128:(i + 1) * 128], vsrc[dil][0][:, ksel(dil, 4 * g + i)], ident, True, True,
                       r=[vsrc[dil][1], "cb"], w=["ps4"], inc=(i == 3))
                actop(Vb[di][:, 4 * g:4 * g + 4, :].rearrange("p b k -> p (b k)"), ps[4], AF.Identity, r=["ps4"], w=["V%d" % di])

        ckpt("b2", [(Vb[0].rearrange("p b k -> p (b k)"), "V0", 0), (Vb[1].rearrange("p b k -> p (b k)"), "V1", 128),
                    (Vb[2].rearrange("p b k -> p (b k)"), "V2", 256), (kT4, "kT4", 384), (kT16, "kT16", 512)])

        def score_tile(bank, col, ncol, ks, qs, nm, dil=1):
            mm(ps[bank][:, col:col + ncol], ksrc[dil][0][:, ks], qTh[:, qs], True, False, r=[ksrc[dil][1], "qTh"], w=["ps%d" % bank], inc=False)
            mm(ps[bank][:, col:col + ncol], ident, nm, False, True, r=["cb"], w=["ps%d" % bank])

        def pv_tile(col, ncol, pbuf, pcol, vblk, first, last, di):
            mm(ps[2][:, col:col + ncol], Vb[di][:, vblk, :], pbuf[:, pcol:pcol + ncol], first, last, r=["V%d" % di, "pb"], w=["ps2"], inc=False)
            mm(ps[3][:, col:col + ncol], ones, pbuf[:, pcol:pcol + ncol], first, last, r=["cb", "pb"], w=["ps3"])

        for g in range(2):
            for hf in range(2):
                sb = hf
                tiles = []
                for qi in range(2):
                    n = 8 + 4 * g + 2 * hf + qi
                    for t, kb in enumerate((n - 1, n)):
                        col = (qi * 2 + t) * 128
                        nm = (nm_pf if n == 8 else nm_prev) if t == 0 else nm_cur
                        score_tile(sb, col, 128, ksel(1, kb), slice((n - 8) * 128, (n - 7) * 128), nm)
                        tiles.append((qi, t, kb, col))
                actop(pb[hf], ps[sb], AF.Exp, r=["ps%d" % sb], w=["pb"], scale=SC)
                for (qi, t, kb, col) in tiles:
                    pv_tile((2 * hf + qi) * 128, 128, pb[hf], col, kb, t == 0, t == 1, 0)
            actop(num[:, g * 512:(g + 1) * 512], ps[2], AF.Identity, r=["ps2"], w=["num"])
            dve(lambda e: e.tensor_copy(out=den[:, g * 512:(g + 1) * 512], in_=ps[3]), ["ps3"], ["den"])
        ckpt("b3", [(num, "num", 0), (den, "den", 128)])
        for n in (2, 3):
            for hf in range(2):
                sb = hf
                tiles = []
                for ci in range(2):
                    c = 2 * hf + ci
                    for t, kn in enumerate((n - 1, n)):
                        col = (ci * 2 + t) * 128
                        nm = (nm_pf if n == 2 else nm_prev) if t == 0 else nm_cur
                        score_tile(sb, col, 128, ksel(4, 4 * kn + c), slice(512 * (n - 2) + c, 512 * (n - 2) + 512, 4), nm, 4)
                        tiles.append((c, t, 4 * kn + c, col))
                actop(pb[hf], ps[sb], AF.Exp, r=["ps%d" % sb], w=["pb"], scale=SC)
                for (c, t, vblk, col) in tiles:
                    pv_tile(c * 128, 128, pb[hf], col, vblk, t == 0, t == 1, 1)
            o0 = 512 * (n - 2)
            nv = num[:, o0:o0 + 512].rearrange("p (j c) -> p c j", c=4)
            dv = den[:, o0:o0 + 512].rearrange("p (j c) -> p c j", c=4)
            dve(lambda e: e.tensor_tensor(out=nv, in0=nv, in1=ps[2].rearrange("p (c j) -> p c j", c=4), op=ALU.add), ["num", "ps2"], ["num"])
            dve(lambda e: e.tensor_tensor(out=dv, in0=dv, in1=ps[3].rearrange("p (c j) -> p c j", c=4), op=ALU.add), ["den", "ps3"], ["den"])
        ckpt("b4", [(num, "num", 0), (den, "den", 128)])
        for rnd in range(2):
            sb = rnd
            for cc in range(8):
                c = 8 * rnd + cc
                score_tile(sb, cc * 64, 64, ksel(16, c), slice(c, 1024, 16), nm16, 16)
            actop(pb[rnd], ps[sb], AF.Exp, r=["ps%d" % sb], w=["pb"], scale=SC)
            for cc in range(8):
                pv_tile(cc * 64, 64, pb[rnd], cc * 64, 8 * rnd + cc, True, True, 2)
            nv = num.rearrange("p (j c) -> p c j", c=16)[:, 8 * rnd:8 * rnd + 8, :]
            dv = den.rearrange("p (j c) -> p c j", c=16)[:, 8 * rnd:8 * rnd + 8, :]
            dve(lambda e: e.tensor_tensor(out=nv, in0=nv, in1=ps[2].rearrange("p (c j) -> p c j", c=8), op=ALU.add), ["num", "ps2"], ["num"])
            dve(lambda e: e.tensor_tensor(out=dv, in0=dv, in1=ps[3].rearrange("p (c j) -> p c j", c=8), op=ALU.add), ["den", "ps3"], ["den"])
        ckpt("b5", [(num, "num", 0), (den, "den", 128)])
        dve(lambda e: e.reciprocal(out=den, in_=den), ["den"], ["den"])
        dve(lambda e: e.tensor_tensor(out=num, in0=num, in1=den, op=ALU.mult), ["num", "den"], ["num"])
        actop(mixT[:, h, :], num, AF.Identity, r=["num"], w=["mix%d" % h])
        for tb in range(2):
            actop(pb[tb], num[:, tb * 512:(tb + 1) * 512], AF.Square, r=["num"], w=["pb"])
            mm(ps[6 + tb], ones, pb[tb], h == 0, h == 7, r=["cb", "pb"], w=["ps%d" % (6 + tb)])
    rs = alloc(1024)
    for tb in range(2):
        actop(rs[:, tb * 512:(tb + 1) * 512], ps[6 + tb], AF.Sqrt, r=["ps%d" % (6 + tb)], w=["rs"], scale=1.0 / 1024, bias=EPS)
    dve(lambda e: e.reciprocal(out=rs, in_=rs), ["rs"], ["rs"])
    for h in range(8):
        dve(lambda e: e.scalar_tensor_tensor(out=mixT[:, h, :], in0=mixT[:, h, :], scalar=pv[:, 40 + h:41 + h],
                                             op0=ALU.mult, in1=rs, op1=ALU.mult), ["mix%d" % h, "pv", "rs"], ["mix%d" % h])
    P.barrier()
    if DEBUG_STAGE == "s1b":
        for j in range(8):
            P.dma("pool", y[j * 128:(j + 1) * 128, 0:1024], mixT[:, j, :], reads=["mix%d" % j], is_out=True)
        for t in P.out_toks:
            P._wait("sp", t)
        return nc
    st["off"] = mark0 + 16 * 1024 // 2

    hres = alloc(8 * 2048).rearrange("p (t f) -> p t f", t=8)
    for tt in range(8):
        P.dma("sp", hres[:, tt, :], d["xo"][tt * 128:(tt + 1) * 128, :], writes=["hres%d" % tt])
    lnp = alloc(4096)
    P.dma("sp", lnp, d["prow"][:, 0:4096].to_broadcast([128, 4096]), writes=["lnp"])
    wo = [alloc(8192, BF16).rearrange("p (c f) -> p c f", c=16) for _ in range(2)]
    mark2 = st["off"]
    for db in range(4):
        P.dma("pool", wo[db % 2].rearrange("p c f -> p (c f)"), d["wout"][db], writes=["wo%d" % (db % 2)])
        for tt in range(8):
            bank = tt % 4
            for c in range(16):
                mm(ps[bank], mixT[:, c, tt * 128:(tt + 1) * 128], wo[db % 2][:, c, :], c == 0, c == 15,
                   r=["mix%d" % c, "wo%d" % (db % 2)], w=["ps%d" % bank], inc=(c == 15))
            hs = hres[:, tt, db * 512:(db + 1) * 512]
            dve(lambda e: e.scalar_tensor_tensor(out=hs, in0=hs, scalar=ALPHA, op0=ALU.mult, in1=ps[bank], op1=ALU.add),
                ["hres%d" % tt, "ps%d" % bank], ["hres%d" % tt])

    bst = alloc(24); mv = alloc(2); rstd = alloc(1)

    def layernorm(xt, key, g, b):
        for k in range(4):
            dve(lambda e: e.bn_stats(out=bst[:, 6 * k:6 * k + 6], in_=xt[:, k * 512:(k + 1) * 512]), [key], ["bst"])
        dve(lambda e: e.bn_aggr(out=mv, in_=bst), ["bst"], ["mv"])
        actop(rstd, mv[:, 1:2], AF.Sqrt, r=["mv"], w=["rstd"], bias=EPS)
        dve(lambda e: e.reciprocal(out=rstd, in_=rstd), ["rstd"], ["rstd"])
        dve(lambda e: e.tensor_scalar(out=xt, in0=xt, scalar1=mv[:, 0:1], scalar2=rstd[:, 0:1],
                                      op0=ALU.subtract, op1=ALU.mult), [key, "mv", "rstd"], [key])
        dve(lambda e: e.tensor_tensor(out=xt, in0=xt, in1=g, op=ALU.mult), [key, "lnp"], [key])
        dve(lambda e: e.tensor_tensor(out=xt, in0=xt, in1=b, op=ALU.add), [key, "lnp"], [key])

    for tt in range(8):
        layernorm(hres[:, tt, :], "hres%d" % tt, lnp[:, 0:2048], lnp[:, 2048:4096])
        if DEBUG_STAGE != "h1":
            P.dma("sp", h1s[tt * 128:(tt + 1) * 128, :], hres[:, tt, :], reads=["hres%d" % tt], writes=["h1s%d" % tt])
    P.barrier()

    if DEBUG_STAGE == "h1":
        for tt in range(8):
            P.dma("sp", y[tt * 128:(tt + 1) * 128, :], hres[:, tt, :], reads=["hres%d" % tt], is_out=True)
        for t in P.out_toks:
            P._wait("sp", t)
        return nc

    st["off"] = mark0
    psb = ps[7].bitcast(BF16)
    lnp = alloc(4096)
    P.dma("sp", lnp, d["prow"][:, 4096:8192].to_broadcast([128, 4096]), writes=["lnp"])
    bst = alloc(24); mv = alloc(2); rstd = alloc(1)
    NB = 1024 // TOKB
    TPB = TOKB // 128
    NG = 128 // GE
    hblk = [alloc(2048) for _ in range(TPB)]
    h1T = alloc(16 * TOKB, BF16).rearrange("p (c t) -> p c t", c=16)
    stab = [alloc(2048).rearrange("p (c k) -> p c k", c=16) for _ in range(TPB)]
    negs1 = [alloc(1024).rearrange("p (h k) -> p h k", h=8) for _ in range(TPB)]
    thr = [alloc(8) for _ in range(TPB)]
    ebias = [alloc(8) for _ in range(TPB)]
    ut = [alloc(2048, BF16).rearrange("p (c j) -> p c j", c=16) for _ in range(2 * GE)]
    vt = [alloc(2048, BF16) for _ in range(2 * GE)]
    Mb = [[[alloc(GE * 128, BF16) for _ in range(8)] for _ in range(TPB)] for _ in range(2)]
    WT = alloc(GE * TOKB, BF16).rearrange("p (e t) -> p e t", e=GE)
    markA = st["off"]
    h1b = alloc(2048, BF16)
    wq = [alloc(2048, BF16).rearrange("p (c j) -> p c j", c=16) for _ in range(2)]
    qT32 = alloc(TOKB)
    v16 = alloc(256).rearrange("p (c k) -> p c k", c=16)
    wk = alloc(256)
    cand = alloc(2048)
    c16 = alloc(128).rearrange("p (h k) -> p h k", h=8)
    d16 = alloc(128).rearrange("p (h k) -> p h k", h=8)
    zz = alloc(8); lnz = alloc(8)
    st["off"] = markA
    Sx = [alloc(GE * 128) for _ in range(2)]
    Eb = [alloc(GE * 128, BF16) for _ in range(2)]
    gx = alloc(GE * TOKB).rearrange("p (e t) -> p e t", e=GE)
    gz = alloc(GE * TOKB).rearrange("p (e t) -> p e t", e=GE)
    gs = alloc(GE * TOKB).rearrange("p (e t) -> p e t", e=GE)

    def load_group(gidx):
        g = gidx % NG
        for ec in range(GE):
            slot = (gidx % 2) * GE + ec
            i1 = g * GE + ec
            P.dma("pool", ut[slot].rearrange("p c j -> p (c j)"), d["uk"][i1], writes=["ut%d" % slot])
            P.dma("pool", vt[slot], d["v"][i1 * 128:(i1 + 1) * 128, :], writes=["vt%d" % slot])

    def gating_ops(g, par):
        ops = []
        for tt in range(TPB):
            for h in range(8):
                def f(tt=tt, h=h):
                    sx = Sx[h % 2]; eb = Eb[h % 2]
                    s2 = stab[tt][:, 2 * h + 1, :].unsqueeze(1).to_broadcast([128, GE, 128])
                    n1 = negs1[tt][:, h, g * GE:(g + 1) * GE].unsqueeze(2).to_broadcast([128, GE, 128])
                    sxv = sx.rearrange("p (e k) -> p e k", e=GE)
                    dve(lambda e: e.tensor_tensor(out=sxv, in0=s2, in1=n1, op=ALU.subtract),
                        ["stab%d" % tt, "negs1_%d" % tt], ["sx%d" % (h % 2)])
                    actop(eb, sx, AF.Exp, r=["sx%d" % (h % 2), "ebias%d" % tt], w=["eb%d" % (h % 2)], bias=ebias[tt][:, h:h + 1])
                    dve(lambda e: e.scalar_tensor_tensor(out=Mb[par][tt][h], in0=sx, scalar=thr[tt][:, h:h + 1], op0=ALU.is_ge,
                                                         in1=eb, op1=ALU.mult),
                        ["sx%d" % (h % 2), "eb%d" % (h % 2), "thr%d" % tt], ["M%d_%d_%d" % (par, tt, h)])
                ops.append(f)
        return ops

    gcount = 0
    load_group(0)
    for kb in range(NB):
        for tt in range(TPB):
            T = kb * TPB + tt
            key = "hb%d" % tt
            P.dma("sp", hblk[tt], h1s[T * 128:(T + 1) * 128, :], reads=["h1s%d" % T], writes=[key])
            actop(h1b, hblk[tt], AF.Identity, r=[key], w=["h1b"])
            for g4 in range(4):
                for i in range(4):
                    c = 4 * g4 + i
                    mm(ps[7][:, i * 128:(i + 1) * 128], h1b[:, c * 128:(c + 1) * 128], ident, True, True,
                       r=["h1b", "cb"], w=["ps7"], inc=(i == 3))
                dve(lambda e: e.tensor_copy(out=h1T[:, 4 * g4:4 * g4 + 4, tt * 128:(tt + 1) * 128],
                                            in_=ps[7].rearrange("p (c t) -> p c t", c=4)), ["ps7"], ["h1T"])
            dve(lambda e: e.tensor_scalar(out=hblk[tt], in0=hblk[tt], scalar1=ALPHA, scalar2=None, op0=ALU.mult), [key], [key])
        P.dma("pool", wq[0].rearrange("p c j -> p (c j)"), d["wqk"][0], writes=["wq0"])
        for ct in range(16):
            if ct + 1 < 16:
                P.dma("pool", wq[(ct + 1) % 2].rearrange("p c j -> p (c j)"), d["wqk"][ct + 1], writes=["wq%d" % ((ct + 1) % 2)])
            for c in range(16):
                mm(ps[6][:, 0:TOKB], wq[ct % 2][:, c, :], h1T[:, c, :], c == 0, c == 15, r=["wq%d" % (ct % 2), "h1T"], w=["ps6"], inc=(c == 15))
            actop(qT32, ps[6][:, 0:TOKB], AF.Identity, r=["ps6"], w=["qT32"])
            for tt in range(TPB):
                mm(ps[5][:, tt * 128:(tt + 1) * 128], qT32[:, tt * 128:(tt + 1) * 128], kT32[:, (ct % 2) * 128:(ct % 2 + 1) * 128], True, True,
                   r=["qT32", "kT32"], w=["ps5"])
            for tt in range(TPB):
                actop(stab[tt][:, ct, :], ps[5][:, tt * 128:(tt + 1) * 128], AF.Identity, r=["ps5"], w=["stab%d" % tt])
        for tt in range(TPB):
            sk = "stab%d" % tt
            for ct in range(16):
                dve(lambda e: e.max(out=v16[:, ct, 0:8], in_=stab[tt][:, ct, :]), [sk], ["v16"])
                dve(lambda e: e.match_replace(out=wk[:, 0:128], in_to_replace=v16[:, ct, 0:8], in_values=stab[tt][:, ct, :], imm_value=-1e30),
                    [sk, "v16"], ["wk"])
                dve(lambda e: e.max(out=v16[:, ct, 8:16], in_=wk[:, 0:128]), ["wk"], ["v16"])
            v16v = v16.rearrange("p (h s) k -> p h s k", s=2)
            candv = cand.rearrange("p (h a b) -> p h a b", h=8, a=16)
            dve(lambda e: e.tensor_tensor(out=candv, in0=v16v[:, :, 0, :].unsqueeze(3).to_broadcast([128, 8, 16, 16]),
                                          in1=v16v[:, :, 1, :].unsqueeze(2).to_broadcast([128, 8, 16, 16]), op=ALU.add), ["v16"], ["cand"])
            for h in range(8):
                ch = cand[:, h * 256:(h + 1) * 256]
                dve(lambda e: e.max(out=c16[:, h, 0:8], in_=ch), ["cand"], ["c16"])
                dve(lambda e: e.match_replace(out=wk, in_to_replace=c16[:, h, 0:8], in_values=ch, imm_value=-1e30), ["cand", "c16"], ["wk"])
                dve(lambda e: e.max(out=c16[:, h, 8:16], in_=wk), ["wk"], ["c16"])
            dve(lambda e: e.tensor_copy(out=thr[tt], in_=c16[:, :, 15]), ["c16"], ["thr%d" % tt])
            dve(lambda e: e.tensor_tensor(out=d16, in0=c16, in1=c16[:, :, 0:1].to_broadcast([128, 8, 16]), op=ALU.subtract), ["c16"], ["d16"])
            actop(d16, d16, AF.Exp, r=["d16"], w=["d16"])
            dve(lambda e: e.tensor_reduce(out=zz, in_=d16, axis=AX.X, op=ALU.add), ["d16"], ["zz"])
            actop(lnz, zz, AF.Ln, r=["zz"], w=["lnz"])
            dve(lambda e: e.scalar_tensor_tensor(out=ebias[tt], in0=c16[:, :, 0], scalar=-1.0, op0=ALU.mult, in1=lnz, op1=ALU.subtract),
                ["c16", "lnz"], ["ebias%d" % tt])
            dve(lambda e: e.tensor_scalar(out=negs1[tt], in0=stab[tt].rearrange("p (h s) k -> p h s k", s=2)[:, :, 0, :],
                                          scalar1=-1.0, scalar2=None, op0=ALU.mult), [sk], ["negs1_%d" % tt])
        P.barrier()
        for f in gating_ops(0, gcount % 2):
            f()
        for g in range(NG):
            par = gcount % 2
            if gcount + 1 < NB * NG:
                load_group(gcount + 1)
            nxt = gating_ops(g + 1, (gcount + 1) % 2) if g + 1 < NG else []
            for ec in range(GE):
                slot = par * GE + ec
                bank = ec % 2
                for c in range(16):
                    mm(ps[bank][:, 0:TOKB], ut[slot][:, c, :], h1T[:, c, :], c == 0, c == 15, r=["ut%d" % slot, "h1T"], w=["ps%d" % bank], inc=(c == 15))
                actop(gx[:, ec, :], ps[bank][:, 0:TOKB], AF.Identity, r=["ps%d" % bank], w=["gx"])
                actop(gz[:, ec, :], ps[bank][:, 0:TOKB], AF.Square, r=["ps%d" % bank], w=["gz"])
            gxf = gx.rearrange("p e t -> p (e t)"); gzf = gz.rearrange("p e t -> p (e t)"); gsf = gs.rearrange("p e t -> p (e t)")
            dve(lambda e: e.tensor_scalar(out=gzf, in0=gzf, scalar1=0.044715, scalar2=1.0, op0=ALU.mult, op1=ALU.add), ["gz"], ["gz"])
            dve(lambda e: e.tensor_tensor(out=gzf, in0=gzf, in1=gxf, op=ALU.mult), ["gz", "gx"], ["gz"])
            actop(gsf, gzf, AF.Sigmoid, r=["gz"], w=["gs"], scale=1.5957691216057308)
            dve(lambda e: e.tensor_tensor(out=gxf, in0=gxf, in1=gsf, op=ALU.mult), ["gx", "gs"], ["gx"])
            for tt in range(TPB):
                bank = 2 + tt % 2
                for ec in range(GE):
                    for h in range(8):
                        mm(ps[bank][:, ec * 128:(ec + 1) * 128], Mb[par][tt][h][:, ec * 128:(ec + 1) * 128], ident, h == 0, h == 7,
                           r=["M%d_%d_%d" % (par, tt, h), "cb"], w=["ps%d" % bank], inc=(h == 7))
                dve(lambda e: e.tensor_tensor(out=WT[:, :, tt * 128:(tt + 1) * 128], in0=ps[bank].rearrange("p (e t) -> p e t", e=4)[:, 0:GE, :],
                                              in1=gx[:, :, tt * 128:(tt + 1) * 128], op=ALU.mult), ["ps%d" % bank, "gx"], ["WT"])
            k = 0
            for tt in range(TPB):
                for db in range(4):
                    bank = 4 + (k % 2)
                    for ec in range(GE):
                        slot = par * GE + ec
                        mm(ps[bank], WT[:, ec, tt * 128:(tt + 1) * 128], vt[slot][:, db * 512:(db + 1) * 512], ec == 0, ec == GE - 1,
                           r=["WT", "vt%d" % slot], w=["ps%d" % bank], inc=(ec == GE - 1))
                    rem = TPB * 4 - k
                    npop = (len(nxt) + rem - 1) // rem if nxt else 0
                    for _ in range(npop):
                        nxt.pop(0)()
                    hs = hblk[tt][:, db * 512:(db + 1) * 512]
                    dve(lambda e: e.tensor_tensor(out=hs, in0=hs, in1=ps[bank], op=ALU.add), ["hb%d" % tt, "ps%d" % bank], ["hb%d" % tt])
                    k += 1
            for f in nxt:
                f()
            gcount += 1
        for tt in range(TPB):
            T = kb * TPB + tt
            layernorm(hblk[tt], "hb%d" % tt, lnp[:, 0:2048], lnp[:, 2048:4096])
            P.dma("sp", y[T * 128:(T + 1) * 128, :], hblk[tt], reads=["hb%d" % tt], is_out=True)
        P.barrier()
    for t in P.out_toks:
        P._wait("sp", t)
    return nc


def _ktile(W):
    K, N = W.shape
    return np.ascontiguousarray(W.reshape(16, 128, N // 128, 128).transpose(2, 1, 0, 3).reshape(N // 128, 128, 2048))


def _consts(half):
    cm = np.zeros((128, NCM), np.float32)
    k = np.arange(128)[:, None]; q = np.arange(128)[None, :]
    cm[:, 0:128] = (k == q)
    cm[:, 128:256] = (k == (q + 64) % 128)
    cm[:, 256:384] = 1.0
    cm[:, 384:512] = np.where(k <= q, 0.0, NEG)
    prev = np.where(k >= q, 0.0, NEG)
    cm[:, 512:640] = prev
    cm[:, 640:768] = prev if half == 1 else NEG
    jq = 64 + np.arange(64)[None, :]
    valid = (k <= jq) & ((k >= 64) | (half == 1))
    cm[:, 768:832] = np.where(valid, 0.0, NEG)
    return cm


_NC_CACHE = {}


def kernel(x, positions, w_in, conv_w, conv_b, rg_w_a, rg_b_a, rg_w_x, rg_b_x, lru_lambda,
           gn_attn, gn_rec, w_out, ln1_g, ln1_b, peer_wq, peer_keys1, peer_keys2,
           peer_u, peer_v, ln2_g, ln2_b):
    f = lambda a: np.asarray(a)
    x = f(x); positions = f(positions)
    key = DEBUG_STAGE
    if key not in _NC_CACHE:
        _NC_CACHE[key] = build()
    nc = _NC_CACHE[key]
    col = lambda vec: np.asarray(vec, np.float32).reshape(8, 128).T
    win = _ktile(f(w_in)[0])
    wout = np.ascontiguousarray(f(w_out)[0].reshape(16, 128, 4, 512).transpose(2, 1, 0, 3).reshape(4, 128, 8192))
    wqk = _ktile(f(peer_wq)[0])
    uk = _ktile(np.ascontiguousarray(f(peer_u)[0].T)) if DEBUG_STAGE is None else None
    v = np.ascontiguousarray(f(peer_v)[0]) if DEBUG_STAGE is None else None
    kT = np.ascontiguousarray(np.concatenate([f(peer_keys1)[0].T, f(peer_keys2)[0].T], axis=1))
    rgw = np.ascontiguousarray(np.stack([f(rg_w_a)[0], f(rg_w_x)[0]], 0).transpose(2, 0, 1, 3).reshape(128, 2048))
    prow = np.concatenate([f(ln1_g)[0], f(ln1_b)[0], f(ln2_g)[0], f(ln2_b)[0]])[None, :].astype(np.float32)
    inv = (10000.0 ** (-(np.arange(64, dtype=np.float32)) / 64.0)).astype(np.float32)
    in_maps = []
    for core in range(8):
        b, half = core // 2, core % 2
        xb = x[b]
        if half == 1:
            xT = np.ascontiguousarray(xb.T)
            pos = positions[b][None, :].astype(np.int32)
        else:
            xT = np.zeros((2048, 2048), np.float32)
            xT[:, 1024:] = xb[:1024].T
            pos = np.zeros((1, 2048), np.int32)
            pos[0, 1024:] = positions[b][:1024]
        pvec = np.zeros((128, NV), np.float32)
        pvec[:, 0:8] = col(f(conv_b)[0]); pvec[:, 8:16] = col(f(rg_b_a)[0]); pvec[:, 16:24] = col(f(rg_b_x)[0])
        pvec[:, 24:32] = col(f(lru_lambda)[0]); pvec[:, 32:40] = col(f(gn_rec)[0]); pvec[:, 40:48] = col(f(gn_attn)[0])
        pvec[:, 48:80] = f(conv_w)[0].reshape(4, 8, 128).transpose(2, 1, 0).reshape(128, 32)
        pvec[:, 80] = np.concatenate([inv, inv])
        sign = np.concatenate([-np.ones(64, np.float32), np.ones(64, np.float32)])
        pvec[:, 81] = sign; pvec[:, 82] = float(half); pvec[:, 83] = 6.28318 * sign
        in_maps.append({"xT": xT, "xo": np.ascontiguousarray(xb[half * 1024:(half + 1) * 1024]), "pos": pos,
                        "win": win, "wout": wout, "wqk": wqk, "uk": uk, "v": v, "kT": kT, "rgw": rgw,
                        "pvec": pvec, "prow": prow, "cmat": _consts(half)})
    if DEBUG_STAGE is not None:
        for m in in_maps:
            m.pop("uk"); m.pop("v")
    res = run_bass_kernel_spmd(nc, in_maps, core_ids=list(range(8)))
    out = np.zeros((4, 2048, 2048), np.float32)
    for core in range(8):
        b, half = core // 2, core % 2
        out[b, half * 1024:(half + 1) * 1024] = res.results[core]["y"]
    return out
```
